# Optimizing a Trainium2 kernel written in Bass

```python
import jax, jax.numpy as jnp
from jax import lax
import numpy as np


D_MODEL = 2048
BATCH = 8
SEQ = 2048
DEPTH = 1

POOL_GROUPS = 4
POOL_GROUP_DIM = 256
POOL_WIDTH = POOL_GROUPS * POOL_GROUP_DIM
POOL_WINDOWS = (2, 4, 8, 16)
N_HEADS = 8
HEAD_DIM = 128
V_HEAD_DIM = 2 * HEAD_DIM
QK_WIDTH = N_HEADS * 2 * HEAD_DIM
ATTN_WIDTH = N_HEADS * V_HEAD_DIM
ROT_DIM = HEAD_DIM // 4
ROT_HALF = ROT_DIM // 2
ROPE_THETA = 500000.0
Q_BLOCK = 128
ATTN_SCALE = HEAD_DIM ** -0.5
SUBLN_EPS = 1e-5
IN_WIDTH = POOL_WIDTH + 2 * QK_WIDTH + ATTN_WIDTH + 2 * D_MODEL
IN_SPLITS = (POOL_WIDTH,
             POOL_WIDTH + QK_WIDTH,
             POOL_WIDTH + 2 * QK_WIDTH,
             POOL_WIDTH + 2 * QK_WIDTH + ATTN_WIDTH,
             POOL_WIDTH + 2 * QK_WIDTH + ATTN_WIDTH + D_MODEL)
N_EXPERTS = 64
EXPERT_DIM = 512
SHARED_DIM = 512
TOP_K = 8
N_GROUPS = 8
TOPK_GROUPS = 4
EXPERTS_PER_GROUP = N_EXPERTS // N_GROUPS
ROUTED_SCALE = 2.5
MOE_BLOCK = 128
N_MOD = 6
NORM_EPS = 1e-6

kernel_name = "hybrid_pool_diffattn_moe_adaln_block"


def rms_norm(x, w, eps=NORM_EPS):
    xf = x.astype(jnp.float32)
    y = xf * lax.rsqrt(jnp.mean(xf * xf, axis=-1, keepdims=True) + eps)
    return (y * w.astype(jnp.float32)).astype(x.dtype)


def rotary_tables(seq_len):
    pos = jnp.arange(seq_len, dtype=jnp.float32)
    inv_freq = 1.0 / (ROPE_THETA ** (jnp.arange(0, ROT_DIM, 2, dtype=jnp.float32) / ROT_DIM))
    ang = pos[:, None] * inv_freq[None, :]
    return jnp.cos(ang), jnp.sin(ang)


def partial_rope(t, cos, sin):
    tf = t.astype(jnp.float32)
    r1 = tf[..., :ROT_HALF]
    r2 = tf[..., ROT_HALF:ROT_DIM]
    cb = cos[:, None, None, :]
    sb = sin[:, None, None, :]
    out = jnp.concatenate([r1 * cb - r2 * sb, r2 * cb + r1 * sb, tf[..., ROT_DIM:]], axis=-1)
    return out.astype(t.dtype)


def pool_mixer(u, pool_w, pool_scale):
    b_, s_, _ = u.shape
    ug = u.reshape(b_, s_, POOL_GROUPS, POOL_GROUP_DIM).astype(jnp.float32)
    cs = jnp.concatenate([jnp.zeros_like(ug[:, :1]), jnp.cumsum(ug, axis=1)], axis=1)
    t = jnp.arange(s_)[:, None]
    win = jnp.array(POOL_WINDOWS, dtype=jnp.int32)[None, :]
    lo = jnp.maximum(t + 1 - win, 0)
    cnt = jnp.minimum(t + 1, win).astype(jnp.float32)
    cs_lo = cs[:, lo, jnp.arange(POOL_GROUPS)[None, :], :]
    mean = (cs[:, 1:] - cs_lo) / cnt[None, :, :, None]
    mixed = (mean - ug).astype(u.dtype)
    y = jnp.einsum('bsgc,gce->bsge', mixed, pool_w) * pool_scale
    return y.reshape(b_, s_, POOL_WIDTH)


def diff_attention(q, k, v, lq1, lk1, lq2, lk2, subln_w, lambda_init, cos, sin):
    b_, s_, _ = q.shape
    q = partial_rope(q.reshape(b_, s_, N_HEADS, 2, HEAD_DIM), cos, sin)
    k = partial_rope(k.reshape(b_, s_, N_HEADS, 2, HEAD_DIM), cos, sin)
    v = v.reshape(b_, s_, N_HEADS, V_HEAD_DIM)
    lam = (jnp.exp(jnp.sum(lq1.astype(jnp.float32) * lk1.astype(jnp.float32)))
           - jnp.exp(jnp.sum(lq2.astype(jnp.float32) * lk2.astype(jnp.float32)))
           + lambda_init)
    nq = s_ // Q_BLOCK
    q_blocks = jnp.moveaxis(q.reshape(b_, nq, Q_BLOCK, N_HEADS, 2, HEAD_DIM), 1, 0)
    q_pos = jnp.arange(s_).reshape(nq, Q_BLOCK)
    k_pos = jnp.arange(s_)

    def attend(args):
        qb, qp = args
        s = jnp.einsum('bqhcd,bkhcd->cbhqk', qb, k, preferred_element_type=jnp.float32) * ATTN_SCALE
        s = jnp.where(qp[:, None] >= k_pos[None, :], s, -jnp.inf)
        p = jax.nn.softmax(s, axis=-1)
        a = (p[0] - lam * p[1]).astype(v.dtype)
        return jnp.einsum('bhqk,bkhe->bqhe', a, v)

    o = lax.map(attend, (q_blocks, q_pos))
    o = jnp.moveaxis(o, 0, 1).reshape(b_, s_, N_HEADS, V_HEAD_DIM)
    o = rms_norm(o, subln_w, SUBLN_EPS) * (1.0 - lambda_init)
    return o.reshape(b_, s_, ATTN_WIDTH)


def hybrid_mixer(h, w_in, pool_w, pool_scale, w_branch_pool, lq1, lk1, lq2, lk2,
                 subln_w, w_branch_attn, w_out, lambda_init, cos, sin):
    proj = jnp.einsum('bsd,de->bse', h, w_in)
    u, q, k, v, g_pool, g_attn = jnp.split(proj, IN_SPLITS, axis=-1)
    y_pool = jnp.einsum('bsp,pd->bsd', pool_mixer(u, pool_w, pool_scale), w_branch_pool)
    y_attn = jnp.einsum('bsa,ad->bsd',
                        diff_attention(q, k, v, lq1, lk1, lq2, lk2, subln_w, lambda_init, cos, sin),
                        w_branch_attn)
    merged = jax.nn.sigmoid(g_pool) * y_pool + jax.nn.sigmoid(g_attn) * y_attn
    return jnp.einsum('bsd,de->bse', merged, w_out)


def swiglu(x, wg, wu, wd):
    return (jax.nn.silu(x @ wg) * (x @ wu)) @ wd


def moe_ffn(h, w_router, router_bias, w_gate_e, w_up_e, w_down_e, w_sh_gate, w_sh_up, w_sh_down):
    b_, s_, d_ = h.shape
    n = b_ * s_
    xf = h.reshape(n, d_)
    scores = jax.nn.sigmoid(jnp.einsum('nd,de->ne', xf, w_router, preferred_element_type=jnp.float32))
    biased = scores + router_bias.astype(jnp.float32)
    grp_score = lax.top_k(biased.reshape(n, N_GROUPS, EXPERTS_PER_GROUP), 2)[0].sum(-1)
    _, top_groups = lax.top_k(grp_score, TOPK_GROUPS)
    group_mask = jnp.any(top_groups[..., None] == jnp.arange(N_GROUPS), axis=-2)
    expert_mask = jnp.repeat(group_mask, EXPERTS_PER_GROUP, axis=-1)
    _, expert_idx = lax.top_k(jnp.where(expert_mask, biased, -jnp.inf), TOP_K)
    sel = jnp.take_along_axis(scores, expert_idx, axis=-1)
    gates = sel / jnp.sum(sel, axis=-1, keepdims=True) * ROUTED_SCALE

    n_assign = n * TOP_K
    n_slots = (n_assign + N_EXPERTS * (MOE_BLOCK - 1) + MOE_BLOCK - 1) // MOE_BLOCK * MOE_BLOCK
    n_blocks = n_slots // MOE_BLOCK
    e_flat = expert_idx.reshape(-1)
    tok_flat = jnp.repeat(jnp.arange(n, dtype=jnp.int32), TOP_K)
    g_flat = gates.reshape(-1)
    order = jnp.argsort(e_flat)
    e_sorted = e_flat[order]
    counts = jnp.bincount(e_flat, length=N_EXPERTS)
    padded = (counts + MOE_BLOCK - 1) // MOE_BLOCK * MOE_BLOCK
    pad_end = jnp.cumsum(padded)
    pad_start = pad_end - padded
    start = jnp.cumsum(counts) - counts
    dest = pad_start[e_sorted] + (jnp.arange(n_assign) - start[e_sorted])
    slot_tok = jnp.full((n_slots,), n, jnp.int32).at[dest].set(tok_flat[order])
    slot_gate = jnp.zeros((n_slots,), jnp.float32).at[dest].set(g_flat[order])
    block_expert = jnp.minimum(
        jnp.searchsorted(pad_end, jnp.arange(n_blocks) * MOE_BLOCK, side='right'), N_EXPERTS - 1)
    x_pad = jnp.concatenate([xf, jnp.zeros((1, d_), xf.dtype)], axis=0)

    def expert_block(args):
        toks, gts, e = args
        xb = x_pad[toks]
        yb = swiglu(xb, w_gate_e[e], w_up_e[e], w_down_e[e])
        return yb * gts[:, None].astype(yb.dtype)

    y_slots = lax.map(expert_block, (slot_tok.reshape(n_blocks, MOE_BLOCK),
                                     slot_gate.reshape(n_blocks, MOE_BLOCK), block_expert))
    routed = jnp.zeros((n + 1, d_), xf.dtype).at[slot_tok].add(y_slots.reshape(n_slots, d_))[:n]
    shared = swiglu(xf, w_sh_gate, w_sh_up, w_sh_down)
    return (shared + routed).reshape(b_, s_, d_)


def setup_inputs(seed: int = 0) -> dict:
    key = jax.random.key(seed)
    ks = jax.random.split(key, 26)
    L, D = DEPTH, D_MODEL

    def nrm(k, shape, scale):
        return jax.random.normal(k, shape, jnp.float32) * scale

    return {
        "x": nrm(ks[0], (BATCH, SEQ, D), 1.0),
        "c": nrm(ks[1], (BATCH, D), 1.0),
        "w_ada": nrm(ks[2], (L, D, N_MOD * D), 0.5 * D ** -0.5),
        "b_ada": nrm(ks[3], (L, N_MOD * D), 0.02),
        "norm1_w": 1.0 + nrm(ks[4], (L, D), 0.05),
        "w_in": nrm(ks[5], (L, D, IN_WIDTH), D ** -0.5),
        "pool_w": nrm(ks[6], (L, POOL_GROUPS, POOL_GROUP_DIM, POOL_GROUP_DIM), POOL_GROUP_DIM ** -0.5),
        "pool_scale": 1.0 + nrm(ks[7], (L, POOL_GROUPS, POOL_GROUP_DIM), 0.1),
        "w_branch_pool": nrm(ks[8], (L, POOL_WIDTH, D), POOL_WIDTH ** -0.5),
        "lambda_q1": nrm(ks[9], (L, HEAD_DIM), 0.1),
        "lambda_k1": nrm(ks[10], (L, HEAD_DIM), 0.1),
        "lambda_q2": nrm(ks[11], (L, HEAD_DIM), 0.1),
        "lambda_k2": nrm(ks[12], (L, HEAD_DIM), 0.1),
        "subln_w": 1.0 + nrm(ks[13], (L, V_HEAD_DIM), 0.05),
        "w_branch_attn": nrm(ks[14], (L, ATTN_WIDTH, D), ATTN_WIDTH ** -0.5),
        "w_out": nrm(ks[15], (L, D, D), D ** -0.5),
        "norm2_w": 1.0 + nrm(ks[16], (L, D), 0.05),
        "w_router": nrm(ks[17], (L, D, N_EXPERTS), D ** -0.5),
        "router_bias": nrm(ks[18], (L, N_EXPERTS), 0.01),
        "w_gate_e": nrm(ks[19], (L, N_EXPERTS, D, EXPERT_DIM), D ** -0.5),
        "w_up_e": nrm(ks[20], (L, N_EXPERTS, D, EXPERT_DIM), D ** -0.5),
        "w_down_e": nrm(ks[21], (L, N_EXPERTS, EXPERT_DIM, D), EXPERT_DIM ** -0.5),
        "w_sh_gate": nrm(ks[22], (L, D, SHARED_DIM), D ** -0.5),
        "w_sh_up": nrm(ks[23], (L, D, SHARED_DIM), D ** -0.5),
        "w_sh_down": nrm(ks[24], (L, SHARED_DIM, D), SHARED_DIM ** -0.5),
        "final_norm_w": 1.0 + nrm(ks[25], (D,), 0.05),
    }


def reference(x, c, w_ada, b_ada, norm1_w, w_in, pool_w, pool_scale, w_branch_pool,
              lambda_q1, lambda_k1, lambda_q2, lambda_k2, subln_w, w_branch_attn, w_out,
              norm2_w, w_router, router_bias, w_gate_e, w_up_e, w_down_e,
              w_sh_gate, w_sh_up, w_sh_down, final_norm_w):
    cos, sin = rotary_tables(x.shape[1])
    c_act = jax.nn.silu(c)
    for l in range(DEPTH):
        lambda_init = 0.8 - 0.6 * float(np.exp(-0.3 * l))
        mod = jnp.einsum('bd,de->be', c_act, w_ada[l]) + b_ada[l]
        sh1, sc1, g1, sh2, sc2, g2 = jnp.split(mod[:, None, :], N_MOD, axis=-1)
        h = rms_norm(x, norm1_w[l]) * (1.0 + sc1) + sh1
        x = x + g1 * hybrid_mixer(h, w_in[l], pool_w[l], pool_scale[l], w_branch_pool[l],
                                  lambda_q1[l], lambda_k1[l], lambda_q2[l], lambda_k2[l],
                                  subln_w[l], w_branch_attn[l], w_out[l], lambda_init, cos, sin)
        h = rms_norm(x, norm2_w[l]) * (1.0 + sc2) + sh2
        x = x + g2 * moe_ffn(h, w_router[l], router_bias[l], w_gate_e[l], w_up_e[l], w_down_e[l],
                             w_sh_gate[l], w_sh_up[l], w_sh_down[l])
    return rms_norm(x, final_norm_w)
```

```python
import os
from contextlib import ExitStack

import ml_dtypes
import numpy as np

import concourse.bass as bass
import concourse.mybir as mybir
from concourse.bass_utils import run_bass_kernel_spmd

F32 = mybir.dt.float32
BF16 = mybir.dt.bfloat16
AF = mybir.ActivationFunctionType
ALU = mybir.AluOpType
AX = mybir.AxisListType

D = 2048
S = 2048
NCH = 16
NTT = 16
NE = 64
EPS = 1e-6
SUBLN_EPS = 1e-5
ATTN_SCALE = 128 ** -0.5
LAMBDA_INIT = 0.2
SAME_ENGINE_SYNC = True

USED_INPUTS = set()

U0, Q0, K0, V0, GP0, GA0 = 0, 1024, 3072, 5120, 7168, 9216


class _Op:
    __slots__ = ("eng", "fn", "deps", "dma", "val", "needs_inc", "n")


class Prog:
    def __init__(self):
        self.ops = []
        self.lastw = {}
        self.readers = {}

    def add(self, eng, fn, r=(), w=(), dma=None):
        o = _Op()
        o.eng, o.fn, o.dma, o.val, o.needs_inc, o.n = eng, fn, dma, 0, False, len(self.ops)
        psk = [k for k in r if isinstance(k, tuple) and k[0] in ("ps", "pst")]
        if psk:
            r = [k for k in r if k not in psk]
            w = list(w) + psk
        deps = {}
        for k in r:
            p = self.lastw.get(k)
            if p is not None:
                deps[p.n] = p
        for k in w:
            p = self.lastw.get(k)
            if p is not None:
                deps[p.n] = p
            rd = self.readers.get(k)
            if rd:
                for q in rd.values():
                    deps[q.n] = q
        o.deps = list(deps.values())
        for k in r:
            rd = self.readers.setdefault(k, {})
            rd[("dma", o.n) if dma is not None else eng] = o
        for k in w:
            self.lastw[k] = o
            self.readers[k] = {}
        self.ops.append(o)
        return o

    def emit(self, nc):
        ops = self.ops
        for o in ops:
            for p in o.deps:
                if p.dma is not None:
                    continue
                if p.eng == o.eng and o.dma is None:
                    if p.eng == "pe" or not SAME_ENGINE_SYNC:
                        continue
                p.needs_inc = True
        cnt = {}
        for o in ops:
            if o.dma is not None:
                key = ("dma", o.dma)
                cnt[key] = cnt.get(key, 0) + 16
                o.val = cnt[key]
            elif o.needs_inc:
                cnt[o.eng] = cnt.get(o.eng, 0) + 1
                o.val = cnt[o.eng]
        with ExitStack() as es:
            sems = {}
            for key in cnt:
                nm = "s_" + "_".join(str(x) for x in (key if isinstance(key, tuple) else (key,)))
                nm = nm.replace("(", "").replace(")", "").replace(",", "_").replace(" ", "").replace("'", "")
                sems[key] = es.enter_context(nc.semaphore(nm))
            block = es.enter_context(nc.Block())

            def run(engname, e):
                waited = {}
                for o in ops:
                    if o.eng != engname:
                        continue
                    for p in o.deps:
                        if p.dma is not None:
                            key = ("dma", p.dma)
                        else:
                            if p.eng == o.eng and o.dma is None and (p.eng == "pe" or not SAME_ENGINE_SYNC):
                                continue
                            key = p.eng
                        if waited.get(key, 0) >= p.val:
                            continue
                        e.wait_ge(sems[key], p.val)
                        waited[key] = p.val
                    if o.fn is None:
                        continue
                    ins = o.fn(e)
                    if o.dma is not None:
                        ins.then_inc(sems[("dma", o.dma)], 16)
                    elif o.needs_inc:
                        ins.then_inc(sems[o.eng], 1)

            @block.tensor
            def _(e):
                run("pe", e)

            @block.scalar
            def _(e):
                run("act", e)

            @block.vector
            def _(e):
                run("dve", e)

            @block.gpsimd
            def _(e):
                run("pool", e)

            @block.sync
            def _(e):
                run("sp", e)


class Rot:
    def __init__(self, items):
        self.items = list(items)
        self.i = 0

    def next(self):
        v = self.items[self.i % len(self.items)]
        self.i += 1
        return v


def wsrc(w2d, r0, nr, c0, ncol):
    return w2d[r0:r0 + nr, c0:c0 + ncol].rearrange("(k p) n -> p k n", p=128)


def build_nc(debug=False, n_experts_run=NE + 1, stop_after=99, substop=9, ne_alloc=NE + 1):
    nc = bass.Bass("TRN2", target_bir_lowering=False)
    dt = nc.dram_tensor

    USED_INPUTS.clear()

    class _Lazy:
        def __init__(self, name, shape, dtype):
            self.name, self.shape, self.dtype, self._ap = name, list(shape), dtype, None

        def ap(self):
            if self._ap is None:
                self._ap = dt(self.name, self.shape, self.dtype, kind="ExternalInput").ap()
                USED_INPUTS.add(self.name)
            return self._ap

        def __getitem__(self, k):
            return self.ap()[k]

        def partition_broadcast(self, n):
            return self.ap().partition_broadcast(n)

        def rearrange(self, *a, **kw):
            return self.ap().rearrange(*a, **kw)

    def inp(name, shape, dtype=F32):
        return _Lazy(name, shape, dtype)

    x_d = inp("x", [S, D])
    cT_d = inp("cT", [128, NCH])
    w_ada = inp("w_ada", [D, 6 * D])
    b_adaT_d = inp("b_adaT", [128, 96])
    b_ada_row = inp("b_ada_row", [1, 6 * D])
    n1T_d = inp("n1T", [128, NCH])
    n2T_d = inp("n2T", [128, NCH])
    fnw_d = inp("fnw", [1, D])
    w_in = inp("w_in", [D, 11264])
    pool_w_d = inp("pool_w", [1024, 256])
    pool_scT_d = inp("pool_scT", [128, 8])
    w_bp = inp("w_bp", [1024, D])
    w_ba = inp("w_ba", [D, D])
    w_out = inp("w_out", [D, D])
    lam_d = inp("lamv", [1, 512])
    sublnT_d = inp("sublnT", [128, 2])
    w_router = inp("w_router", [D, NE])
    rbias_d = inp("rbias", [1, NE])
    wg_d = inp("w_gate_e", [ne_alloc, D, 512])
    wu_d = inp("w_up_e", [ne_alloc, D, 512])
    wd_d = inp("w_down_e", [ne_alloc, 512, D])
    ident_d = inp("ident", [128, 128], BF16)
    identf_d = inp("identf", [128, 128], F32)
    ropeC_d = inp("ropeC", [32, S], BF16)
    ropeS_d = inp("ropeS", [32, S], BF16)
    rotR_d = inp("rotR", [32, 32], BF16)
    cmask_d = inp("cmask", [128, 128], BF16)
    ratio_d = inp("ratio", [128, 4 * 16])

    y_d = dt("y", [S, D], F32, kind="ExternalOutput").ap()
    okind = "ExternalOutput" if debug else "Internal"
    onT_d = dt("onT_d", [NCH, 128, S], BF16, kind=okind).ap()
    x2_d = dt("x2_d", [S, D], F32, kind=okind).ap()
    if debug:
        hT_dbg = dt("hT_dbg", [NCH, 128, S], BF16, kind="ExternalOutput").ap()
        mod_dbg = dt("mod_dbg", [128, 96], F32, kind="ExternalOutput").ap()
        g_dbg = dt("g_dbg", [2, 128, D], F32, kind="ExternalOutput").ap()
        gates_dbg = dt("gates_dbg", [NTT, 128, NE + 1], F32, kind="ExternalOutput").ap()

    with ExitStack() as top:
        def sb(name, shape, dtype=F32, stack=top):
            return stack.enter_context(nc.sbuf_tensor("sb_" + name, list(shape), dtype))

        def psum(name, shape, dtype=F32, stack=top):
            return stack.enter_context(nc.psum_tensor("pp_" + name, list(shape), dtype))

        ps = [psum(f"ps{i}", [128, 512]) for i in range(6)]
        pst = psum("pst", [128, 8, 128], F32)
        ident = sb("ident", [128, 128], BF16)
        A1 = sb("A1", [128, NCH])
        B1 = sb("B1", [128, NCH])
        A2 = sb("A2", [128, NCH])
        B2 = sb("B2", [128, NCH])
        g1b = sb("g1b", [128, D])
        g2b = sb("g2b", [128, D])
        lam = sb("lam", [128, 1])
        nlam = sb("nlam", [128, 1])

        def build_hT_tile(P, src_tile_ap, src_keys, A, B, dstT, dst_col0, dst_keyf, tmp, tag):
            xsq, ss, rstd, xn = tmp["xsq"], tmp["ss"], tmp["rstd"], tmp["xn"]
            P.add("dve", lambda e: e.scalar_tensor_tensor(
                out=xsq[:], in0=src_tile_ap, scalar=1.0, in1=src_tile_ap,
                op0=ALU.mult, op1=ALU.mult, accum_out=ss[:, 0:1]),
                r=src_keys, w=[("xsq", tag), ("ss", tag)])
            P.add("act", lambda e: e.activation(out=rstd[:, 0:1], in_=ss[:, 0:1], func=AF.Ln,
                                                 scale=1.0 / D, bias=tmp["eps"][:, 0:1]),
                  r=[("ss", tag)], w=[("rstd", tag)])
            P.add("act", lambda e: e.activation(out=rstd[:, 0:1], in_=rstd[:, 0:1], func=AF.Exp, scale=-0.5),
                  r=[("rstd", tag)], w=[("rstd", tag)])
            P.add("act", lambda e: e.activation(out=xn[:], in_=src_tile_ap, func=AF.Copy, scale=rstd[:, 0:1]),
                  r=list(src_keys) + [("rstd", tag)], w=[("xn", tag)])
            for g in range(4):
                bank = tmp["pst_rot"].next()
                for j in range(4):
                    c = g * 4 + j
                    P.add("pe", lambda e, c=c, bank=bank, j=j: e.matmul(
                        pst[:, bank * 4 + j, :], xn[:, c * 128:(c + 1) * 128], ident[:], start=True, stop=True),
                        r=[("xn", tag), "ident"], w=[("pst", bank)])
                for j in range(4):
                    c = g * 4 + j
                    slot = bank * 4 + j
                    dst = dstT[:, c, dst_col0:dst_col0 + 128]
                    if bank == 0:
                        P.add("act", lambda e, c=c, slot=slot, dst=dst: e.activation(
                            out=dst, in_=pst[:, slot, :], func=AF.Identity,
                            scale=A[:, c:c + 1], bias=B[:, c:c + 1]),
                            r=["AB"], w=[("pst", bank), dst_keyf(c)])
                    else:
                        P.add("dve", lambda e, c=c, slot=slot, dst=dst: e.tensor_scalar(
                            out=dst, in0=pst[:, slot, :], scalar1=A[:, c:c + 1], scalar2=B[:, c:c + 1],
                            op0=ALU.mult, op1=ALU.add),
                            r=["AB"], w=[("pst", bank), dst_keyf(c)])

        with ExitStack() as ph:
            P = Prog()
            cT = sb("cT", [128, NCH], F32, ph)
            cact = sb("cact", [128, NCH], BF16, ph)
            cb = sb("cb", [128, NCH, 128], BF16, ph)
            wr = [sb(f"wr0_{i}", [128, NCH, 512], BF16, ph) for i in range(2)]
            modT = sb("modT", [128, 96], F32, ph)
            badT = sb("badT", [128, 96], F32, ph)
            n1T = sb("n1T", [128, NCH], F32, ph)
            n2T = sb("n2T", [128, NCH], F32, ph)
            bb = sb("bb", [128, D], F32, ph)
            lv = sb("lv", [128, 512], F32, ph)
            lprod = sb("lprod", [128, 128], F32, ph)
            ls = sb("ls", [128, 2], F32, ph)
            tmp16 = sb("tmp16", [128, NCH], F32, ph)
            psm = ps[4]

            P.add("sp", lambda e: e.dma_start(out=ident[:], in_=ident_d[:, :]), w=["ident"], dma="c0")
            P.add("sp", lambda e: e.dma_start(out=cT[:], in_=cT_d[:, :]), w=["cT"], dma="c1")
            P.add("sp", lambda e: e.dma_start(out=badT[:], in_=b_adaT_d[:, :]), w=["badT"], dma="c2")
            P.add("sp", lambda e: e.dma_start(out=n1T[:], in_=n1T_d[:, :]), w=["n1T"], dma="c3")
            P.add("sp", lambda e: e.dma_start(out=n2T[:], in_=n2T_d[:, :]), w=["n2T"], dma="c4")
            P.add("sp", lambda e: e.dma_start(out=lv[:], in_=lam_d.partition_broadcast(128)), w=["lv"], dma="c5")
            P.add("act", lambda e: e.activation(out=cact[:], in_=cT[:], func=AF.Silu), r=["cT"], w=["cact"])
            P.add("dve", lambda e: e.tensor_copy(out=cb[:], in_=cact[:].unsqueeze(2).to_broadcast([128, NCH, 128])),
                  r=["cact"], w=["cb"])
            for j in range(2):
                P.add("dve", lambda e, j=j: e.scalar_tensor_tensor(
                    out=lprod[:], in0=lv[:, (2 * j) * 128:(2 * j + 1) * 128], scalar=1.0,
                    in1=lv[:, (2 * j + 1) * 128:(2 * j + 2) * 128], op0=ALU.mult, op1=ALU.mult,
                    accum_out=ls[:, j:j + 1]), r=["lv"], w=["lprod", ("ls", j)])
            P.add("act", lambda e: e.activation(out=ls[:], in_=ls[:], func=AF.Exp),
                  r=[("ls", 0), ("ls", 1)], w=[("ls", 0), ("ls", 1)])
            P.add("dve", lambda e: e.tensor_tensor(out=lam[:], in0=ls[:, 0:1], in1=ls[:, 1:2], op=ALU.subtract),
                  r=[("ls", 0), ("ls", 1)], w=["lam"])
            P.add("dve", lambda e: e.tensor_scalar(out=lam[:], in0=lam[:], scalar1=LAMBDA_INIT, scalar2=None,
                                                   op0=ALU.add), r=["lam"], w=["lam"])
            P.add("dve", lambda e: e.tensor_scalar(out=nlam[:], in0=lam[:], scalar1=-1.0, scalar2=None,
                                                   op0=ALU.mult), r=["lam"], w=["nlam"])

            order = [0, 1, 2, 3, 4, 5, 6, 7, 12, 13, 14, 15, 16, 17, 18, 19, 8, 9, 10, 11, 20, 21, 22, 23]
            psrot = Rot([0, 1, 2, 3])
            for i, blk in enumerate(order):
                slot = i % 2
                P.add("pool", lambda e, blk=blk, slot=slot: e.dma_start(
                    out=wr[slot][:], in_=wsrc(w_ada, 0, D, blk * 512, 512)),
                    w=[("wr", slot)], dma=("wr", slot))
                if blk in (8, 9, 10, 11, 20, 21, 22, 23):
                    gi = 0 if blk < 12 else 1
                    gb = g1b if gi == 0 else g2b
                    cbk = (blk - 8) if gi == 0 else (blk - 20)
                    if cbk == 0:
                        c0 = 8 * 512 if gi == 0 else 20 * 512
                        P.add("sp", lambda e, c0=c0: e.dma_start(
                            out=bb[:], in_=b_ada_row[0:1, c0:c0 + D].partition_broadcast(128)),
                            w=["bb"], dma="bb")
                    bank = psrot.next()
                    for k in range(NCH):
                        P.add("pe", lambda e, k=k, slot=slot, bank=bank: e.matmul(
                            ps[bank][:], cb[:, k, :], wr[slot][:, k, :], start=(k == 0), stop=(k == NCH - 1)),
                            r=["cb", ("wr", slot)], w=[("ps", bank)])
                    P.add("dve", lambda e, bank=bank, gb=gb, cbk=cbk: e.tensor_tensor(
                        out=gb[:, cbk * 512:(cbk + 1) * 512], in0=ps[bank][:], in1=bb[:, cbk * 512:(cbk + 1) * 512],
                        op=ALU.add), r=[("ps", bank), "bb"], w=[("gb", gi, cbk)])
                else:
                    for sub in range(4):
                        jc = blk * 4 + sub
                        for k in range(NCH):
                            P.add("pe", lambda e, k=k, slot=slot, sub=sub, jc=jc: e.matmul(
                                psm[:, jc:jc + 1], wr[slot][:, k, sub * 128:(sub + 1) * 128], cact[:, k:k + 1],
                                start=(k == 0), stop=(k == NCH - 1)),
                                r=["cact", ("wr", slot)], w=["psm"])
            P.add("dve", lambda e: e.tensor_tensor(out=modT[:], in0=psm[:, 0:96], in1=badT[:], op=ALU.add),
                  r=["psm", "badT"], w=["modT"])
            P.add("dve", lambda e: e.tensor_scalar(out=tmp16[:], in0=modT[:, 16:32], scalar1=1.0, scalar2=None,
                                                   op0=ALU.add), r=["modT"], w=["tmp16"])
            P.add("dve", lambda e: e.tensor_tensor(out=A1[:], in0=tmp16[:], in1=n1T[:], op=ALU.mult),
                  r=["tmp16", "n1T"], w=["AB"])
            P.add("dve", lambda e: e.tensor_copy(out=B1[:], in_=modT[:, 0:16]), r=["modT"], w=["AB1"])
            P.add("dve", lambda e: e.tensor_scalar(out=tmp16[:], in0=modT[:, 64:80], scalar1=1.0, scalar2=None,
                                                   op0=ALU.add), r=["modT", "AB"], w=["tmp16"])
            P.add("dve", lambda e: e.tensor_tensor(out=A2[:], in0=tmp16[:], in1=n2T[:], op=ALU.mult),
                  r=["tmp16", "n2T"], w=["AB2"])
            P.add("dve", lambda e: e.tensor_copy(out=B2[:], in_=modT[:, 48:64]), r=["modT"], w=["AB3"])
            fin_r = ["AB", "AB1", "AB2", "AB3", "lam", "nlam", "ident"] + [("gb", gi, c) for gi in range(2) for c in range(4)]
            if debug:
                P.add("sp", lambda e: e.dma_start(out=mod_dbg[:, :], in_=modT[:]), r=["modT"], w=["d0"], dma="d0")
                P.add("sp", lambda e: e.dma_start(out=g_dbg[0], in_=g1b[:]), r=fin_r, w=["d1"], dma="d1")
                P.add("sp", lambda e: e.dma_start(out=g_dbg[1], in_=g2b[:]), r=fin_r, w=["d2"], dma="d2")
                fin_r = fin_r + ["d0", "d1", "d2"]
            P.add("sp", None, r=fin_r)
            import os
            if not os.environ.get('SKIP0'):
                P.emit(nc)
        if stop_after <= 0:
            return nc

        with ExitStack() as ph:
            P = Prog()
            hT = sb("hT", [128, NCH, S], BF16, ph)
            xt = [sb(f"xt{i}", [128, D], F32, ph) for i in range(2)]
            tmpn = dict(xsq=sb("xsq", [128, D], BF16, ph), ss=sb("ss", [128, 1], F32, ph),
                        rstd=sb("rstd", [128, 1], F32, ph), xn=sb("xn", [128, D], BF16, ph),
                        eps=sb("epsn", [128, 1], F32, ph), pst_rot=Rot(range(2)))
            P.add("dve", lambda e: e.memset(tmpn["eps"][:], EPS), w=["eps"])
            import os
            for tt in range(int(os.environ.get('NT1', NTT))):
                slot = tt % 2
                P.add("sp", lambda e, tt=tt, slot=slot: e.dma_start(out=xt[slot][:], in_=x_d[tt * 128:(tt + 1) * 128, :]),
                      w=[("xt", slot)], dma=("xt", slot))
                build_hT_tile(P, xt[slot][:], [("xt", slot), "eps"], A1, B1, hT, tt * 128,
                              lambda c, tt=tt: ("hT", c, tt), tmpn, "n1")
            if debug and not os.environ.get('NODBG'):
                for c in range(NCH):
                    P.add("sp", lambda e, c=c: e.dma_start(out=hT_dbg[c], in_=hT[:, c, :]),
                          r=[("hT", c, tt) for tt in range(NTT)], w=[("dbgh", c)], dma=("dbgh", c % 4))

            wq = [sb(f"wq{i}", [128, NCH, 256], BF16, ph) for i in range(4)]
            qT = sb("qT", [128, 2, S], BF16, ph)
            kT = sb("kT", [128, 2, S], BF16, ph)
            vv = sb("vv", [128, NTT, 264], BF16, ph)
            ropeC = sb("ropeC", [32, S], BF16, ph)
            ropeS = sb("ropeS", [32, S], BF16, ph)
            rotR = sb("rotR", [32, 32], BF16, ph)
            cmask = sb("cmask", [128, 128], BF16, ph)
            sublnT = sb("sublnT", [128, 2], F32, ph)
            PT = [sb(f"PT{i}", [128, 512], BF16, ph) for i in range(3)]
            rt1 = sb("rt1", [32, 512], F32, ph)
            rt2 = sb("rt2", [32, 512], F32, ph)
            on1 = sb("on1", [128, 4, 256], F32, ph)
            oo = sb("oo", [128, 4, 256], F32, ph)
            osq = sb("osq", [128, 256], BF16, ph)
            onb = sb("onb", [128, 4, 256], BF16, ph)
            rz = sb("rz", [128, 4], F32, ph)
            rz2 = sb("rz2", [128, 4], F32, ph)
            oss = sb("oss", [128, 4], F32, ph)
            orst = sb("orst", [128, 4], F32, ph)
            eps2 = sb("eps2", [128, 1], F32, ph)
            onh = [sb("onh0", [128, 2, S], BF16, ph)] * 2

            if substop >= 1:
                P.add("sp", lambda e: e.dma_start(out=ropeC[:], in_=ropeC_d[:, :]), w=["ropeC"], dma="k0")
                P.add("sp", lambda e: e.dma_start(out=ropeS[:], in_=ropeS_d[:, :]), w=["ropeS"], dma="k1")
                P.add("sp", lambda e: e.dma_start(out=rotR[:], in_=rotR_d[:, :]), w=["rotR"], dma="k2")
                P.add("sp", lambda e: e.dma_start(out=cmask[:], in_=cmask_d[:, :]), w=["cmask"], dma="k3")
                P.add("sp", lambda e: e.dma_start(out=sublnT[:], in_=sublnT_d[:, :]), w=["sublnT0"], dma="k4")
                P.add("dve", lambda e: e.tensor_scalar(out=sublnT[:], in0=sublnT[:], scalar1=1.0 - LAMBDA_INIT,
                                                       scalar2=None, op0=ALU.mult), r=["sublnT0"], w=["sublnT"])
                P.add("dve", lambda e: e.memset(eps2[:], SUBLN_EPS), w=["eps2"])
                for tt_ in range(NTT):
                    P.add("dve", lambda e, tt_=tt_: e.memset(vv[:, tt_, 256:258], 1.0), w=["vones"])
            wslot_rot = Rot(range(4))
            proj_rot = Rot([4, 5])
            st_rot = Rot([4, 5])
            pt_rot = Rot(range(3))
            pstr = tmpn["pst_rot"]

            def load_head_w(hd):
                slots = []
                for j, c0 in enumerate((Q0 + hd * 256, K0 + hd * 256, V0 + hd * 256)):
                    sl = wslot_rot.next()
                    P.add("pool", lambda e, sl=sl, c0=c0: e.dma_start(out=wq[sl][:], in_=wsrc(w_in, 0, D, c0, 256)),
                          w=[("wq", sl)], dma=("wq", sl))
                    slots.append(sl)
                return slots

            def hT_keys(k, tb):
                return [("hT", k, tb * 4 + j) for j in range(4)]

            def project_head(hd, slots):
                sq, sk, sv = slots
                for (dstT, sl, scale, nm) in ((qT, sq, ATTN_SCALE, "q"), (kT, sk, 1.0, "k")):
                    for ci in range(2):
                        for tb in range(4):
                            bank = proj_rot.next()
                            for k in range(NCH):
                                P.add("pe", lambda e, k=k, sl=sl, ci=ci, tb=tb, bank=bank: e.matmul(
                                    ps[bank][:], wq[sl][:, k, ci * 128:(ci + 1) * 128], hT[:, k, tb * 512:(tb + 1) * 512],
                                    start=(k == 0), stop=(k == NCH - 1)),
                                    r=[("wq", sl)] + hT_keys(k, tb), w=[("ps", bank)])
                            dst = dstT[:, ci, tb * 512:(tb + 1) * 512]
                            P.add("act", lambda e, dst=dst, bank=bank, scale=scale: e.activation(
                                out=dst, in_=ps[bank][:], func=AF.Copy, scale=scale),
                                r=[("ps", bank)], w=[(nm, ci, tb)])
                            bank2 = proj_rot.next()
                            P.add("pe", lambda e, dst=dst, bank2=bank2: e.matmul(
                                ps[bank2][0:32, :], rotR[:, :], dst[0:32, :], start=True, stop=True),
                                r=[(nm, ci, tb), "rotR"], w=[("ps", bank2)])
                            P.add("dve", lambda e, bank2=bank2, tb=tb: e.tensor_tensor(
                                out=rt1[:], in0=ps[bank2][0:32, :], in1=ropeS[:, tb * 512:(tb + 1) * 512], op=ALU.mult),
                                r=[("ps", bank2), "ropeS"], w=["rt1"])
                            P.add("dve", lambda e, dst=dst, tb=tb: e.tensor_tensor(
                                out=rt2[:], in0=dst[0:32, :], in1=ropeC[:, tb * 512:(tb + 1) * 512], op=ALU.mult),
                                r=[(nm, ci, tb), "ropeC"], w=["rt2"])
                            P.add("dve", lambda e, dst=dst: e.tensor_tensor(
                                out=dst[0:32, :], in0=rt1[:], in1=rt2[:], op=ALU.add),
                                r=["rt1", "rt2"], w=[(nm, ci, tb)])
                for tp in range(NTT // 2):
                    bank = proj_rot.next()
                    for j in range(2):
                        tt = tp * 2 + j
                        for k in range(NCH):
                            P.add("pe", lambda e, k=k, tt=tt, j=j, bank=bank: e.matmul(
                                ps[bank][:, j * 256:(j + 1) * 256], hT[:, k, tt * 128:(tt + 1) * 128], wq[sv][:, k, :],
                                start=(k == 0), stop=(k == NCH - 1)),
                                r=[("wq", sv), ("hT", k, tt)], w=[("ps", bank)])
                    P.add("act", lambda e, tp=tp, bank=bank: e.activation(
                        out=vv[:, tp * 2:tp * 2 + 2, 0:256], in_=ps[bank][:].rearrange("p (a b) -> p a b", a=2),
                        func=AF.Copy), r=[("ps", bank)], w=[("v", tp)])

            def attend_head(hd):
                ob = onh[hd % 2]
                for qb in range(4):
                    nkt = 4 * qb + 4
                    for m in range(2):
                        for kt in range(nkt):
                            j0 = max(0, kt - 4 * qb)
                            c0 = j0 * 128
                            ncol = 512 - c0
                            sbk = st_rot.next()
                            P.add("pe", lambda e, m=m, kt=kt, qb=qb, c0=c0, ncol=ncol, sbk=sbk: e.matmul(
                                ps[sbk][:, 0:ncol], kT[:, m, kt * 128:(kt + 1) * 128],
                                qT[:, m, qb * 512 + c0:(qb + 1) * 512], start=True, stop=True),
                                r=[("k", m, kt // 4), ("q", m, qb)], w=[("ps", sbk)])
                            pi = pt_rot.next()
                            P.add("act", lambda e, pi=pi, sbk=sbk, ncol=ncol: e.activation(
                                out=PT[pi][:, 0:ncol], in_=ps[sbk][:, 0:ncol], func=AF.Exp),
                                r=[("ps", sbk)], w=[("PT", pi)])
                            if kt >= 4 * qb:
                                P.add("pool", lambda e, pi=pi: e.tensor_tensor(
                                    out=PT[pi][:, 0:128], in0=PT[pi][:, 0:128], in1=cmask[:], op=ALU.mult),
                                    r=[("PT", pi), "cmask"], w=[("PT", pi)])
                            for j in range(j0, 4):
                                P.add("pe", lambda e, j=j, c0=c0, pi=pi, kt=kt: e.matmul(
                                    ps[j][:, 0:257], PT[pi][:, j * 128 - c0:(j + 1) * 128 - c0], vv[:, kt, 0:257],
                                    start=(kt == 0), stop=(kt == min(nkt - 1, 4 * qb + j))),
                                    r=[("PT", pi), ("v", kt // 2), "vones"], w=[("ps", j)])
                        for j in range(4):
                            if m == 0:
                                P.add("dve", lambda e, j=j: e.reciprocal(out=rz[:, j:j + 1], in_=ps[j][:, 256:257]),
                                      r=[("ps", j)], w=[("rz", j)])
                                P.add("dve", lambda e, j=j: e.tensor_scalar(
                                    out=on1[:, j, :], in0=ps[j][:, 0:256], scalar1=rz[:, j:j + 1], scalar2=None,
                                    op0=ALU.mult), r=[("ps", j), ("rz", j)], w=[("on1", j)])
                            else:
                                P.add("dve", lambda e, j=j: e.reciprocal(out=rz2[:, j:j + 1], in_=ps[j][:, 256:257]),
                                      r=[("ps", j)], w=[("rz2", j)])
                                P.add("dve", lambda e, j=j: e.tensor_scalar(
                                    out=rz2[:, j:j + 1], in0=rz2[:, j:j + 1], scalar1=nlam[:, 0:1], scalar2=None,
                                    op0=ALU.mult), r=[("rz2", j), "nlam"], w=[("rz2", j)])
                                P.add("dve", lambda e, j=j: e.scalar_tensor_tensor(
                                    out=oo[:, j, :], in0=ps[j][:, 0:256], scalar=rz2[:, j:j + 1], in1=on1[:, j, :],
                                    op0=ALU.mult, op1=ALU.add), r=[("ps", j), ("rz2", j), ("on1", j)], w=[("oo", j)])
                                P.add("dve", lambda e, j=j: e.scalar_tensor_tensor(
                                    out=osq[:], in0=oo[:, j, :], scalar=1.0, in1=oo[:, j, :], op0=ALU.mult,
                                    op1=ALU.mult, accum_out=oss[:, j:j + 1]), r=[("oo", j)], w=["osq", ("oss", j)])
                    P.add("act", lambda e: e.activation(out=orst[:], in_=oss[:], func=AF.Ln, scale=1.0 / 256,
                                                         bias=eps2[:, 0:1]),
                          r=[("oss", j) for j in range(4)] + ["eps2"], w=["orst"])
                    P.add("act", lambda e: e.activation(out=orst[:], in_=orst[:], func=AF.Exp, scale=-0.5),
                          r=["orst"], w=["orst"])
                    for j in range(4):
                        P.add("dve", lambda e, j=j: e.tensor_scalar(
                            out=onb[:, j, :], in0=oo[:, j, :], scalar1=orst[:, j:j + 1], scalar2=None, op0=ALU.mult),
                            r=[("oo", j), "orst"], w=[("onb", j)])
                        bank = pstr.next()
                        for ec in range(2):
                            P.add("pe", lambda e, j=j, ec=ec, bank=bank: e.matmul(
                                pst[:, bank * 4 + ec, :], onb[:, j, ec * 128:(ec + 1) * 128], ident[:], start=True, stop=True),
                                r=[("onb", j), "ident"], w=[("pst", bank)])
                        q0 = qb * 512 + j * 128
                        for ec in range(2):
                            P.add("dve", lambda e, ec=ec, bank=bank, q0=q0, ob=ob: e.tensor_scalar(
                                out=ob[:, ec, q0:q0 + 128], in0=pst[:, bank * 4 + ec, :], scalar1=sublnT[:, ec:ec + 1],
                                scalar2=None, op0=ALU.mult),
                                r=["sublnT"], w=[("pst", bank), ("onh", 0, ec, qb)])
                for ec in range(2):
                    P.add("sp", lambda e, ec=ec, ob=ob: e.dma_start(out=onT_d[hd * 2 + ec], in_=ob[:, ec, :]),
                          r=[("onh", 0, ec, qb) for qb in range(4)], w=[("onT_d", hd * 2 + ec)],
                          dma=("onst", ec))

            nheads = 8 if stop_after >= 2 else 1
            if substop < 1:
                nheads = 0
            if nheads:
                nxt = load_head_w(0)
            for hd in range(nheads):
                cur = nxt
                project_head(hd, cur)
                if hd + 1 < nheads:
                    nxt = load_head_w(hd + 1)
                if substop >= 2:
                    attend_head(hd)
            fin = [("onT_d", i) for i in range(nheads * 2)] if substop >= 2 else [("q", 1, 3), ("k", 1, 3), ("v", 7)][:3 * min(nheads, 1)]
            if debug and not os.environ.get('NODBG'):
                fin += [("dbgh", c) for c in range(NCH)]
            P.add("sp", None, r=fin)
            P.emit(nc)
        if stop_after <= 2:
            return nc

        with ExitStack() as ph:
            P = Prog()
            hTb = sb("hTb", [128, NCH, 512], BF16, ph)
            onTb = sb("onTb", [128, NCH, 512], BF16, ph)
            NW = 4
            wr3 = [sb(f"wr3_{i}", [128, 4096], BF16, ph) for i in range(NW)]
            wbp = [sb(f"wbp{i}", [128, 8, 512], BF16, ph) for i in range(2)]
            wbp_rot = Rot(range(2))
            uext = sb("uext", [128, 8, 528], F32, ph)
            pa = sb("pa", [128, 528], F32, ph)
            pb = sb("pb", [128, 528], F32, ph)
            mixT = sb("mixT", [128, 8, 512], BF16, ph)
            pmT = sb("pmT", [128, 8, 512], BF16, ph)
            mergT = sb("mergT", [128, NCH, 512], BF16, ph)
            sg1 = sb("sg1", [128, 512], F32, ph)
            sg2 = sb("sg2", [128, 512], F32, ph)
            mp = sb("mp", [128, 512], F32, ph)
            mq = sb("mq", [128, 512], F32, ph)
            xb = sb("xb", [128, 4, D], F32, ph)
            poolw = sb("poolw", [128, 8, 256], BF16, ph)
            pscT = sb("pscT", [128, 8], F32, ph)
            ratio = sb("ratio", [128, 64], F32, ph)
            tmpn = dict(xsq=sb("xsq3", [128, D], BF16, ph), ss=sb("ss3", [128, 1], F32, ph),
                        rstd=sb("rstd3", [128, 1], F32, ph), xn=sb("xn3", [128, D], BF16, ph),
                        eps=sb("epsn3", [128, 1], F32, ph), pst_rot=Rot(range(2)))
            P.add("dve", lambda e: e.memset(tmpn["eps"][:], EPS), w=["eps"])
            P.add("dve", lambda e: e.memset(uext[:, :, 0:16], 0.0), w=["halo"])
            P.add("sp", lambda e: e.dma_start(out=pscT[:], in_=pool_scT_d[:, :]), w=["pscT"], dma="k0")
            P.add("sp", lambda e: e.dma_start(out=ratio[:], in_=ratio_d[:, :]), w=["ratio"], dma="k1")
            P.add("pool", lambda e: e.dma_start(out=poolw[:], in_=wsrc(pool_w_d, 0, 1024, 0, 256)),
                  w=["poolw"], dma="k2")
            wrot = Rot(range(NW))
            bank_rot = Rot(range(6))

            def wload(src_ap, shape3):
                sl = wrot.next()
                k, n = shape3
                dst = wr3[sl][:, 0:k * n].rearrange("p (k n) -> p k n", k=k)
                P.add("pool", lambda e, dst=dst, src_ap=src_ap: e.dma_start(out=dst, in_=src_ap),
                      w=[("wr3", sl)], dma=("wr3", sl))
                return sl, dst

            nblk = 4
            for tb in range(nblk):
                for j in range(4):
                    tt = tb * 4 + j
                    P.add("sp", lambda e, j=j, tt=tt: e.dma_start(out=xb[:, j, :], in_=x_d[tt * 128:(tt + 1) * 128, :]),
                          w=[("xb", j)], dma=("xb", j))
                    build_hT_tile(P, xb[:, j, :], [("xb", j), "eps"], A1, B1, hTb, j * 128,
                                  lambda c, j=j: ("hTb", c, j), tmpn, "n3")
                P.add("sp", lambda e, tb=tb: e.dma_start(
                    out=onTb[:], in_=onT_d[:, :, tb * 512:(tb + 1) * 512].rearrange("c p n -> p c n")),
                    w=[("onTb", c) for c in range(NCH)], dma="onTb")

                def hTb_keys(k):
                    return [("hTb", k, j) for j in range(4)]

                for un in range(4):
                    sl, wv = wload(wsrc(w_in, 0, D, U0 + un * 256, 256), (NCH, 256))
                    for ci in range(2):
                        c = un * 2 + ci
                        bank = bank_rot.next()
                        for k in range(NCH):
                            P.add("pe", lambda e, k=k, wv=wv, ci=ci, bank=bank: e.matmul(
                                ps[bank][:], wv[:, k, ci * 128:(ci + 1) * 128], hTb[:, k, :],
                                start=(k == 0), stop=(k == NCH - 1)),
                                r=[("wr3", sl)] + hTb_keys(k), w=[("ps", bank)])
                        P.add("act", lambda e, c=c, bank=bank: e.activation(
                            out=uext[:, c, 16:528], in_=ps[bank][:], func=AF.Copy),
                            r=[("ps", bank)], w=[("u", c)])
                        g = c // 2
                        srcb, src_key = None, None
                        cur = uext[:, c, :]
                        cur_keys = [("u", c), "halo"]
                        bufs = [pa, pb]
                        for st_i in range(g + 1):
                            step = 1 << st_i
                            dstb = bufs[st_i % 2]
                            dkey = "pa" if st_i % 2 == 0 else "pb"
                            lo = 2 * step - 1
                            P.add("pool", lambda e, cur=cur, dstb=dstb, step=step, lo=lo: e.tensor_tensor(
                                out=dstb[:, lo:528], in0=cur[:, lo:528], in1=cur[:, lo - step:528 - step], op=ALU.add),
                                r=cur_keys, w=[dkey])
                            cur = dstb[:, :]
                            cur_keys = [dkey]
                        wsz = 1 << (g + 1)
                        if tb == 0:
                            P.add("pool", lambda e, cur=cur, g=g: e.tensor_tensor(
                                out=cur[:, 16:32], in0=cur[:, 16:32], in1=ratio[:, g * 16:(g + 1) * 16], op=ALU.mult),
                                r=cur_keys + ["ratio"], w=cur_keys)
                        P.add("dve", lambda e, cur=cur, c=c, wsz=wsz: e.scalar_tensor_tensor(
                            out=mixT[:, c, :], in0=cur[:, 16:528], scalar=1.0 / wsz, in1=uext[:, c, 16:528],
                            op0=ALU.mult, op1=ALU.subtract), r=cur_keys + [("u", c)], w=[("mix", c)])
                P.add("pool", lambda e: e.tensor_copy(out=uext[:, :, 0:16], in_=uext[:, :, 512:528]),
                      r=[("u", c) for c in range(8)], w=["halo"])
                for g in range(4):
                    for ec in range(2):
                        bank = bank_rot.next()
                        for cc in range(2):
                            P.add("pe", lambda e, g=g, ec=ec, cc=cc, bank=bank: e.matmul(
                                ps[bank][:], poolw[:, g * 2 + cc, ec * 128:(ec + 1) * 128], mixT[:, 2 * g + cc, :],
                                start=(cc == 0), stop=(cc == 1)),
                                r=["poolw", ("mix", 2 * g + cc)], w=[("ps", bank)])
                        P.add("dve", lambda e, g=g, ec=ec, bank=bank: e.tensor_scalar(
                            out=pmT[:, 2 * g + ec, :], in0=ps[bank][:], scalar1=pscT[:, 2 * g + ec:2 * g + ec + 1],
                            scalar2=None, op0=ALU.mult),
                            r=[("ps", bank), "pscT"], w=[("pm", 2 * g + ec)])
                for fp in range(8):
                    if fp % 2 == 0:
                        bpi = wbp_rot.next()
                        w_bpv = wbp[bpi]
                        sl_bp = ("bp", bpi)
                        P.add("pool", lambda e, w_bpv=w_bpv, fp=fp: e.dma_start(
                            out=w_bpv[:], in_=wsrc(w_bp, 0, 1024, (fp // 2) * 512, 512)),
                            w=[("wr3", sl_bp)], dma=("wbp", bpi))
                    sl_gp, w_gpv = wload(wsrc(w_in, 0, D, GP0 + fp * 256, 256), (NCH, 256))
                    sl_ba, w_bav = wload(wsrc(w_ba, 0, D, fp * 256, 256), (NCH, 256))
                    sl_ga, w_gav = wload(wsrc(w_in, 0, D, GA0 + fp * 256, 256), (NCH, 256))
                    for ci in range(2):
                        fc = fp * 2 + ci
                        bA, bB, bC, bD = (bank_rot.next() for _ in range(4))
                        off = ((fp % 2) * 2 + ci) * 128
                        for k in range(8):
                            P.add("pe", lambda e, k=k, off=off, bA=bA, w_bpv=w_bpv: e.matmul(
                                ps[bA][:], w_bpv[:, k, off:off + 128], pmT[:, k, :], start=(k == 0), stop=(k == 7)),
                                r=[("wr3", sl_bp), ("pm", k)], w=[("ps", bA)])
                        for k in range(NCH):
                            P.add("pe", lambda e, k=k, ci=ci, bB=bB, w_gpv=w_gpv: e.matmul(
                                ps[bB][:], w_gpv[:, k, ci * 128:(ci + 1) * 128], hTb[:, k, :],
                                start=(k == 0), stop=(k == NCH - 1)),
                                r=[("wr3", sl_gp)] + hTb_keys(k), w=[("ps", bB)])
                        P.add("act", lambda e, bB=bB: e.activation(out=sg1[:], in_=ps[bB][:], func=AF.Sigmoid),
                              r=[("ps", bB)], w=["sg1"])
                        P.add("dve", lambda e, bA=bA: e.tensor_tensor(out=mp[:], in0=ps[bA][:], in1=sg1[:], op=ALU.mult),
                              r=[("ps", bA), "sg1"], w=["mp"])
                        for k in range(NCH):
                            P.add("pe", lambda e, k=k, ci=ci, bC=bC, w_bav=w_bav: e.matmul(
                                ps[bC][:], w_bav[:, k, ci * 128:(ci + 1) * 128], onTb[:, k, :],
                                start=(k == 0), stop=(k == NCH - 1)),
                                r=[("wr3", sl_ba), ("onTb", k)], w=[("ps", bC)])
                        for k in range(NCH):
                            P.add("pe", lambda e, k=k, ci=ci, bD=bD, w_gav=w_gav: e.matmul(
                                ps[bD][:], w_gav[:, k, ci * 128:(ci + 1) * 128], hTb[:, k, :],
                                start=(k == 0), stop=(k == NCH - 1)),
                                r=[("wr3", sl_ga)] + hTb_keys(k), w=[("ps", bD)])
                        P.add("act", lambda e, bD=bD: e.activation(out=sg2[:], in_=ps[bD][:], func=AF.Sigmoid),
                              r=[("ps", bD)], w=["sg2"])
                        P.add("dve", lambda e, bC=bC: e.tensor_tensor(out=mq[:], in0=ps[bC][:], in1=sg2[:], op=ALU.mult),
                              r=[("ps", bC), "sg2"], w=["mq"])
                        P.add("dve", lambda e, fc=fc: e.tensor_tensor(out=mergT[:, fc, :], in0=mp[:], in1=mq[:], op=ALU.add),
                              r=["mp", "mq"], w=[("merg", fc)])
                for cbk in range(4):
                    sl0, w0 = wload(wsrc(w_out, 0, 1024, cbk * 512, 512), (8, 512))
                    sl1, w1 = wload(wsrc(w_out, 1024, 1024, cbk * 512, 512), (8, 512))
                    for j in range(4):
                        bank = bank_rot.next()
                        for k in range(NCH):
                            wv_, sl_ = (w0, sl0) if k < 8 else (w1, sl1)
                            P.add("pe", lambda e, k=k, j=j, wv_=wv_, bank=bank: e.matmul(
                                ps[bank][:], mergT[:, k, j * 128:(j + 1) * 128], wv_[:, k % 8, :],
                                start=(k == 0), stop=(k == NCH - 1)),
                                r=[("wr3", sl_), ("merg", k)], w=[("ps", bank)])
                        P.add("dve", lambda e, bank=bank, cbk=cbk: e.tensor_tensor(
                            out=mp[:], in0=ps[bank][:], in1=g1b[:, cbk * 512:(cbk + 1) * 512], op=ALU.mult),
                            r=[("ps", bank)], w=["mp"])
                        P.add("pool", lambda e, j=j, cbk=cbk: e.tensor_tensor(
                            out=xb[:, j, cbk * 512:(cbk + 1) * 512], in0=xb[:, j, cbk * 512:(cbk + 1) * 512],
                            in1=mp[:], op=ALU.add), r=["mp", ("xb", j)], w=[("xb", j)])
                for j in range(4):
                    tt = tb * 4 + j
                    P.add("sp", lambda e, j=j, tt=tt: e.dma_start(out=x2_d[tt * 128:(tt + 1) * 128, :], in_=xb[:, j, :]),
                          r=[("xb", j)], w=[("x2_d", tt)], dma=("x2st", j))
            P.add("sp", None, r=[("x2_d", tt) for tt in range(nblk * 4)])
            P.emit(nc)
        if stop_after <= 3:
            return nc

        with ExitStack() as ph:
            P = Prog()
            h2T = sb("h2T", [128, NCH, 1024], BF16, ph)
            yacc = sb("yacc", [128, 8, D], F32, ph)
            NW = 5
            wr4 = [sb(f"wr4_{i}", [128, 4096], BF16, ph) for i in range(NW)]
            actT = sb("actT", [128, 4, 1024], BF16, ph)
            sgt = [sb(f"sgt{i}", [128, 512], F32, ph) for i in range(2)]
            xt = [sb(f"xt4_{i}", [128, D], F32, ph) for i in range(2)]
            wrt = sb("wrt", [128, NCH, NE], BF16, ph)
            rbias = sb("rbias", [128, NE], F32, ph)
            gates = sb("gates", [128, 8, NE + 1], F32, ph)
            fnwb = sb("fnwb", [128, D], F32, ph)
            sc_ = sb("sc_", [128, NE], F32, ph)
            bi_ = sb("bi_", [128, NE], F32, ph)
            eq_ = sb("eq_", [128, NE], F32, ph)
            bi2_ = sb("bi2_", [128, NE], F32, ph)
            mb_ = sb("mb_", [128, NE], F32, ph)
            m1_ = sb("m1_", [128, 8], F32, ph)
            m2_ = sb("m2_", [128, 8], F32, ph)
            gs_ = sb("gs_", [128, 8], F32, ph)
            rk_ = sb("rk_", [128, 8], F32, ph)
            gm_ = sb("gm_", [128, 8], F32, ph)
            mx8 = sb("mx8", [128, 8], F32, ph)
            ssum = sb("ssum", [128, 1], F32, ph)
            tmpn = dict(xsq=sb("xsq4", [128, D], BF16, ph), ss=sb("ss4", [128, 1], F32, ph),
                        rstd=sb("rstd4", [128, 1], F32, ph), xn=sb("xn4", [128, D], BF16, ph),
                        eps=sb("epsn4", [128, 1], F32, ph), pst_rot=Rot(range(2)))
            P.add("dve", lambda e: e.memset(tmpn["eps"][:], EPS), w=["eps"])
            P.add("pool", lambda e: e.dma_start(out=wrt[:], in_=wsrc(w_router, 0, D, 0, NE)), w=["wrt"], dma="k0")
            P.add("sp", lambda e: e.dma_start(out=rbias[:], in_=rbias_d.partition_broadcast(128)), w=["rbias"], dma="k1")
            P.add("sp", lambda e: e.dma_start(out=fnwb[:], in_=fnw_d.partition_broadcast(128)), w=["fnwb"], dma="k2")
            P.add("dve", lambda e: e.memset(gates[:, :, NE:NE + 1], 1.0), w=["gone"])
            wrot = Rot(range(NW))
            gu_rot = Rot([0, 1, 2, 3])
            dn_rot = Rot([4, 5])
            sg_rot = Rot([0, 1])

            def wload4(src_ap, shape3):
                sl = wrot.next()
                k, n = shape3
                dst = wr4[sl][:, 0:k * n].rearrange("p (k n) -> p k n", k=k)
                P.add("pool", lambda e, dst=dst, src_ap=src_ap: e.dma_start(out=dst, in_=src_ap),
                      w=[("wr4", sl)], dma=("wr4", sl))
                return sl, dst

            def v3(ap):
                return ap.rearrange("p (a b) -> p a b", a=8)

            for hf in range(2):
                for j in range(8):
                    tt = hf * 8 + j
                    slot = j % 2
                    P.add("sp", lambda e, tt=tt, slot=slot: e.dma_start(out=xt[slot][:], in_=x2_d[tt * 128:(tt + 1) * 128, :]),
                          w=[("xt", slot)], dma=("xt", slot))
                    build_hT_tile(P, xt[slot][:], [("xt", slot), "eps"], A2, B2, h2T, j * 128,
                                  lambda c, j=j: ("h2T", c, j), tmpn, "n4")
                    bank = dn_rot.next()
                    for k in range(NCH):
                        P.add("pe", lambda e, k=k, j=j, bank=bank: e.matmul(
                            ps[bank][:, 0:NE], h2T[:, k, j * 128:(j + 1) * 128], wrt[:, k, :],
                            start=(k == 0), stop=(k == NCH - 1)),
                            r=["wrt", ("h2T", k, j)], w=[("ps", bank)])
                    P.add("act", lambda e, bank=bank: e.activation(out=sc_[:], in_=ps[bank][:, 0:NE], func=AF.Sigmoid),
                          r=[("ps", bank)], w=["sc_"])
                    P.add("dve", lambda e: e.tensor_tensor(out=bi_[:], in0=sc_[:], in1=rbias[:], op=ALU.add),
                          r=["sc_", "rbias"], w=["bi_"])
                    P.add("dve", lambda e: e.tensor_reduce(out=m1_[:], in_=v3(bi_[:]), axis=AX.X, op=ALU.max),
                          r=["bi_"], w=["m1_"])
                    P.add("dve", lambda e: e.tensor_tensor(out=v3(eq_[:]), in0=v3(bi_[:]),
                                                           in1=m1_[:].unsqueeze(2).to_broadcast([128, 8, 8]),
                                                           op=ALU.is_equal), r=["bi_", "m1_"], w=["eq_"])
                    P.add("dve", lambda e: e.scalar_tensor_tensor(out=bi2_[:], in0=eq_[:], scalar=-4.0, in1=bi_[:],
                                                                  op0=ALU.mult, op1=ALU.add),
                          r=["eq_", "bi_"], w=["bi2_"])
                    P.add("dve", lambda e: e.tensor_reduce(out=m2_[:], in_=v3(bi2_[:]), axis=AX.X, op=ALU.max),
                          r=["bi2_"], w=["m2_"])
                    P.add("dve", lambda e: e.tensor_tensor(out=gs_[:], in0=m1_[:], in1=m2_[:], op=ALU.add),
                          r=["m1_", "m2_"], w=["gs_"])
                    P.add("dve", lambda e: e.tensor_tensor(out=v3(eq_[:]),
                                                           in0=gs_[:].unsqueeze(1).to_broadcast([128, 8, 8]),
                                                           in1=gs_[:].unsqueeze(2).to_broadcast([128, 8, 8]),
                                                           op=ALU.is_gt), r=["gs_", "bi2_"], w=["eq_"])
                    P.add("dve", lambda e: e.tensor_reduce(out=rk_[:], in_=v3(eq_[:]), axis=AX.X, op=ALU.add),
                          r=["eq_"], w=["rk_"])
                    P.add("dve", lambda e: e.tensor_scalar(out=gm_[:], in0=rk_[:], scalar1=3.5, scalar2=None,
                                                           op0=ALU.is_lt), r=["rk_"], w=["gm_"])
                    P.add("dve", lambda e: e.scalar_tensor_tensor(out=v3(mb_[:]), in0=v3(bi_[:]), scalar=2.0,
                                                                  in1=gm_[:].unsqueeze(2).to_broadcast([128, 8, 8]),
                                                                  op0=ALU.add, op1=ALU.mult),
                          r=["bi_", "gm_"], w=["mb_"])
                    P.add("dve", lambda e: e.max(out=mx8[:], in_=mb_[:]), r=["mb_"], w=["mx8"])
                    P.add("dve", lambda e: e.tensor_scalar(out=eq_[:], in0=mb_[:], scalar1=mx8[:, 7:8], scalar2=None,
                                                           op0=ALU.is_ge), r=["mb_", "mx8", "rk_"], w=["eq_"])
                    P.add("dve", lambda e: e.scalar_tensor_tensor(out=bi2_[:], in0=sc_[:], scalar=1.0, in1=eq_[:],
                                                                  op0=ALU.mult, op1=ALU.mult, accum_out=ssum[:, 0:1]),
                          r=["sc_", "eq_", "m2_"], w=["bi2_", "ssum"])
                    P.add("dve", lambda e: e.reciprocal(out=ssum[:], in_=ssum[:]), r=["ssum"], w=["ssum"])
                    P.add("dve", lambda e: e.tensor_scalar(out=ssum[:], in0=ssum[:], scalar1=2.5, scalar2=None, op0=ALU.mult),
                          r=["ssum"], w=["ssum"])
                    P.add("dve", lambda e, j=j: e.tensor_scalar(out=gates[:, j, 0:NE], in0=bi2_[:], scalar1=ssum[:, 0:1],
                                                                scalar2=None, op0=ALU.mult),
                          r=["bi2_", "ssum"], w=[("gates", j)])
                    if debug:
                        P.add("sp", lambda e, j=j, tt=tt: e.dma_start(out=gates_dbg[tt], in_=gates[:, j, :]),
                              r=[("gates", j), "gone"], w=[("gdbg", tt)], dma=("gdbg", j))

                elist = list(range(n_experts_run - 1)) + [NE]
                for ei, ex in enumerate(elist):
                    for jh in range(2):
                        slg, wgv = wload4(wsrc(wg_d[ex], 0, D, jh * 256, 256), (NCH, 256))
                        slu, wuv = wload4(wsrc(wu_d[ex], 0, D, jh * 256, 256), (NCH, 256))
                        for ffc in range(2):
                            f = jh * 2 + ffc
                            for tb in range(2):
                                bG, bU = gu_rot.next(), gu_rot.next()
                                for (bk, wv_, sl_) in ((bG, wgv, slg), (bU, wuv, slu)):
                                    for k in range(NCH):
                                        P.add("pe", lambda e, k=k, bk=bk, wv_=wv_, ffc=ffc, tb=tb: e.matmul(
                                            ps[bk][:], wv_[:, k, ffc * 128:(ffc + 1) * 128], h2T[:, k, tb * 512:(tb + 1) * 512],
                                            start=(k == 0), stop=(k == NCH - 1)),
                                            r=[("wr4", sl_)] + [("h2T", k, tb * 4 + q) for q in range(4)], w=[("ps", bk)])
                                si = sg_rot.next()
                                P.add("act", lambda e, bG=bG, si=si: e.activation(out=sgt[si][:], in_=ps[bG][:], func=AF.Silu),
                                      r=[("ps", bG)], w=[("sgt", si)])
                                P.add("dve", lambda e, bU=bU, si=si, f=f, tb=tb: e.tensor_tensor(
                                    out=actT[:, f, tb * 512:(tb + 1) * 512], in0=ps[bU][:], in1=sgt[si][:], op=ALU.mult),
                                    r=[("ps", bU), ("sgt", si)], w=[("actT", f, tb)])
                    sld = []
                    for kh in range(2):
                        sld.append(wload4(wsrc(wd_d[ex], kh * 256, 256, 0, D), (2, D)))
                    for j in range(8):
                        for cbk in range(4):
                            bank = dn_rot.next()
                            for f in range(4):
                                sl_, wv_ = sld[f // 2]
                                P.add("pe", lambda e, f=f, j=j, cbk=cbk, wv_=wv_, bank=bank: e.matmul(
                                    ps[bank][:], actT[:, f, j * 128:(j + 1) * 128], wv_[:, f % 2, cbk * 512:(cbk + 1) * 512],
                                    start=(f == 0), stop=(f == 3)),
                                    r=[("wr4", sl_), ("actT", f, j // 4)], w=[("ps", bank)])
                            ya = yacc[:, j, cbk * 512:(cbk + 1) * 512]
                            gcol = gates[:, j, ex:ex + 1]
                            if ei == 0:
                                P.add("dve", lambda e, ya=ya, gcol=gcol, bank=bank: e.tensor_scalar(
                                    out=ya, in0=ps[bank][:], scalar1=gcol, scalar2=None, op0=ALU.mult),
                                    r=[("ps", bank), ("gates", j), "gone"], w=[("yacc", j, cbk)])
                            else:
                                P.add("dve", lambda e, ya=ya, gcol=gcol, bank=bank: e.scalar_tensor_tensor(
                                    out=ya, in0=ps[bank][:], scalar=gcol, in1=ya, op0=ALU.mult, op1=ALU.add),
                                    r=[("ps", bank), ("gates", j), "gone", ("yacc", j, cbk)], w=[("yacc", j, cbk)])
                for j in range(8):
                    tt = hf * 8 + j
                    slot = j % 2
                    P.add("sp", lambda e, tt=tt, slot=slot: e.dma_start(out=xt[slot][:], in_=x2_d[tt * 128:(tt + 1) * 128, :]),
                          w=[("xt", slot)], dma=("xt", slot))
                    P.add("pool", lambda e, j=j: e.tensor_tensor(out=yacc[:, j, :], in0=yacc[:, j, :], in1=g2b[:], op=ALU.mult),
                          r=[("yacc", j, c) for c in range(4)], w=[("yacc", j, c) for c in range(4)])
                    P.add("pool", lambda e, j=j, slot=slot: e.tensor_tensor(out=yacc[:, j, :], in0=yacc[:, j, :], in1=xt[slot][:], op=ALU.add),
                          r=[("yacc", j, c) for c in range(4)] + [("xt", slot)], w=[("yacc", j, c) for c in range(4)])
                    P.add("dve", lambda e, j=j: e.scalar_tensor_tensor(
                        out=tmpn["xsq"][:], in0=yacc[:, j, :], scalar=1.0, in1=yacc[:, j, :], op0=ALU.mult, op1=ALU.mult,
                        accum_out=tmpn["ss"][:, 0:1]), r=[("yacc", j, c) for c in range(4)], w=[("xsq", "n4"), ("ss", "n4")])
                    P.add("act", lambda e: e.activation(out=tmpn["rstd"][:, 0:1], in_=tmpn["ss"][:, 0:1], func=AF.Ln,
                                                         scale=1.0 / D, bias=tmpn["eps"][:, 0:1]),
                          r=[("ss", "n4"), "eps"], w=[("rstd", "n4")])
                    P.add("act", lambda e: e.activation(out=tmpn["rstd"][:, 0:1], in_=tmpn["rstd"][:, 0:1], func=AF.Exp, scale=-0.5),
                          r=[("rstd", "n4")], w=[("rstd", "n4")])
                    P.add("dve", lambda e, j=j, slot=slot: e.scalar_tensor_tensor(
                        out=xt[slot][:], in0=yacc[:, j, :], scalar=tmpn["rstd"][:, 0:1], in1=fnwb[:], op0=ALU.mult, op1=ALU.mult),
                        r=[("yacc", j, c) for c in range(4)] + [("rstd", "n4"), "fnwb", ("xt", slot)], w=[("xt", slot)])
                    P.add("sp", lambda e, tt=tt, slot=slot: e.dma_start(out=y_d[tt * 128:(tt + 1) * 128, :], in_=xt[slot][:]),
                          r=[("xt", slot)], w=[("y", tt)], dma=("yst", slot))
            fin = [("y", tt) for tt in range(NTT)]
            if debug:
                fin += [("gdbg", tt) for tt in range(NTT)]
            P.add("sp", None, r=fin)
            P.emit(nc)
    return nc


def _consts():
    bf = ml_dtypes.bfloat16
    ident = np.eye(128, dtype=np.float32)
    pos = np.arange(S, dtype=np.float32)
    inv_freq = (1.0 / (np.float32(500000.0) ** (np.arange(0, 32, 2, dtype=np.float32) / np.float32(32)))).astype(np.float32)
    ang = pos[None, :] * inv_freq[:, None]
    cos, sin = np.cos(ang).astype(np.float32), np.sin(ang).astype(np.float32)
    ropeC = np.concatenate([cos, cos], 0)
    ropeS = np.concatenate([-sin, sin], 0)
    rotR = np.zeros((32, 32), np.float32)
    for m in range(32):
        rotR[(m + 16) % 32, m] = 1.0
    kk = np.arange(128)
    cmask = (kk[None, :] >= kk[:, None]).astype(np.float32)
    ratio = np.ones((128, 64), np.float32)
    for g, w in enumerate((2, 4, 8, 16)):
        t = np.arange(16)
        ratio[:, g * 16:(g + 1) * 16] = (w / np.minimum(t + 1, w)).astype(np.float32)[None, :]
    return dict(ident=ident.astype(bf), identf=ident, ropeC=ropeC.astype(bf), ropeS=ropeS.astype(bf), rotR=rotR.astype(bf),
                cmask=cmask.astype(bf), ratio=ratio)


def _in_maps(inputs, cores):
    f = lambda a: np.ascontiguousarray(np.asarray(a, dtype=np.float32))
    colT = lambda v, n: np.ascontiguousarray(np.asarray(v, np.float32).reshape(n, 128).T)
    cst = _consts()
    shared = dict(
        w_ada=f(inputs["w_ada"][0]), b_adaT=colT(inputs["b_ada"][0], 96), b_ada_row=f(inputs["b_ada"][0])[None, :],
        n1T=colT(inputs["norm1_w"][0], 16), n2T=colT(inputs["norm2_w"][0], 16), fnw=f(inputs["final_norm_w"])[None, :],
        w_in=f(inputs["w_in"][0]), pool_w=f(inputs["pool_w"][0]).reshape(1024, 256),
        pool_scT=colT(inputs["pool_scale"][0].reshape(-1), 8), w_bp=f(inputs["w_branch_pool"][0]),
        w_ba=f(inputs["w_branch_attn"][0]), w_out=f(inputs["w_out"][0]),
        lamv=np.concatenate([f(inputs["lambda_q1"][0]), f(inputs["lambda_k1"][0]),
                             f(inputs["lambda_q2"][0]), f(inputs["lambda_k2"][0])])[None, :],
        sublnT=colT(inputs["subln_w"][0], 2), w_router=f(inputs["w_router"][0]), rbias=f(inputs["router_bias"][0])[None, :],
        w_gate_e=np.concatenate([f(inputs["w_gate_e"][0]), f(inputs["w_sh_gate"][0])[None]], 0),
        w_up_e=np.concatenate([f(inputs["w_up_e"][0]), f(inputs["w_sh_up"][0])[None]], 0),
        w_down_e=np.concatenate([f(inputs["w_down_e"][0]), f(inputs["w_sh_down"][0])[None]], 0),
        **cst,
    )
    maps = []
    for b in cores:
        m = dict(shared)
        m["x"] = f(inputs["x"][b])
        m["cT"] = colT(inputs["c"][b], 16)
        maps.append(m)
    return maps


def kernel(**inputs):
    nc = build_nc()
    maps = [{k: v for k, v in m.items() if k in USED_INPUTS} for m in _in_maps(inputs, range(8))]
    res = run_bass_kernel_spmd(nc, maps, core_ids=list(range(8)))
    return np.stack([np.asarray(r["y"], dtype=np.float32) for r in res.results], axis=0)
```
